# Optimizing a Trainium2 kernel written in Bass

```python
import jax, jax.numpy as jnp
from jax import lax
import numpy as np

D_MODEL = 1024
BATCH = 8
SEQ = 8192
DEPTH = 2

GRID_W = 64
CTX_LEN = 256
N_HEADS = 8
N_KV_HEADS = 2
HEAD_DIM = 64
Q_GROUP = N_HEADS // N_KV_HEADS
ATTN_W = N_HEADS * HEAD_DIM
KV_W = N_KV_HEADS * HEAD_DIM
ROPE_AXIS_DIM = HEAD_DIM // 2
ROPE_BASE = 10000.0
Q_BLOCK = 128
CONV_W = D_MODEL - ATTN_W
MIX_IN_W = ATTN_W + 2 * KV_W + 3 * CONV_W
MIX_OUT_IN = ATTN_W + CONV_W
POOL_WINDOWS = (2, 4, 8, 16)
N_POOL_GROUPS = len(POOL_WINDOWS)
POOL_GROUP_W = D_MODEL // N_POOL_GROUPS
D_FF = 2816
N_EXPERTS = 8
TOP_K = 2
D_FF_EXPERT = 1408
N_MOD = 6
N_EVEN = (DEPTH + 1) // 2
N_ODD = DEPTH // 2
EPS = 1e-6

kernel_name = "hybrid_dit_attn_conv_pool_moe"


def rms_norm(x, g):
    xf = x.astype(jnp.float32)
    y = xf * lax.rsqrt(jnp.mean(xf * xf, axis=-1, keepdims=True) + EPS)
    return (y * g.astype(jnp.float32)).astype(x.dtype)


def modulate(h, shift, scale):
    return h * (1 + scale) + shift


def adaln(cvec, w, b):
    return jnp.split(jax.nn.silu(cvec) @ w + b, N_MOD, axis=-1)


def axial_rope_tables(n_tokens):
    rows = n_tokens // GRID_W
    row = jnp.broadcast_to(jnp.arange(rows)[:, None], (rows, GRID_W)).reshape(-1)
    col = jnp.broadcast_to(jnp.arange(GRID_W)[None, :], (rows, GRID_W)).reshape(-1)
    freqs = ROPE_BASE ** (-jnp.arange(0, ROPE_AXIS_DIM, 2, dtype=jnp.float32) / ROPE_AXIS_DIM)
    ang = jnp.stack([row.astype(jnp.float32)[:, None] * freqs,
                     col.astype(jnp.float32)[:, None] * freqs], axis=1)
    return jnp.cos(ang), jnp.sin(ang)


def apply_axial_rope(x, cos, sin):
    xr = x.astype(jnp.float32).reshape(*x.shape[:-1], 2, 2, ROPE_AXIS_DIM // 2)
    x1, x2 = xr[..., 0, :], xr[..., 1, :]
    c = cos[None, :, None]
    s = sin[None, :, None]
    out = jnp.stack([x1 * c - x2 * s, x1 * s + x2 * c], axis=-2)
    return out.reshape(x.shape).astype(x.dtype)


def attend(qg, k, v):
    s = jnp.einsum('bqkgd,bskd->bkgqs', qg, k, preferred_element_type=jnp.float32) * (HEAD_DIM ** -0.5)
    p = jax.nn.softmax(s, axis=-1).astype(v.dtype)
    return jnp.einsum('bkgqs,bskd->bqkgd', p, v)


def latent_attention(q, k_all, v_all):
    b, n = q.shape[:2]
    nblk = n // Q_BLOCK
    qb = q.reshape(b, nblk, Q_BLOCK, N_KV_HEADS, Q_GROUP, HEAD_DIM).transpose(1, 0, 2, 3, 4, 5)
    o = lax.map(lambda qblk: attend(qblk, k_all, v_all), qb)
    return o.transpose(1, 0, 2, 3, 4, 5).reshape(b, n, ATTN_W)


def short_conv3(u, w):
    up = jnp.pad(u, ((0, 0), (1, 1), (0, 0)))
    return up[:, :-2] * w[0] + up[:, 1:-1] * w[1] + up[:, 2:] * w[2]


def split_mix(z):
    cuts = [ATTN_W, ATTN_W + KV_W, ATTN_W + 2 * KV_W,
            ATTN_W + 2 * KV_W + CONV_W, ATTN_W + 2 * KV_W + 2 * CONV_W]
    return jnp.split(z, cuts, axis=-1)


def attn_conv_mixer(a_lat, a_ctx, w_in, q_g, k_g, conv_w, w_out, rope_cos, rope_sin, ctx_live):
    b, n = a_lat.shape[:2]
    nc = a_ctx.shape[1]
    q_l, k_l, v_l, gb_l, gc_l, u_l = split_mix(a_lat @ w_in)
    q_c, k_c, v_c, gb_c, gc_c, u_c = split_mix(a_ctx @ w_in)
    q_l = apply_axial_rope(rms_norm(q_l.reshape(b, n, N_HEADS, HEAD_DIM), q_g), rope_cos, rope_sin)
    k_l = apply_axial_rope(rms_norm(k_l.reshape(b, n, N_KV_HEADS, HEAD_DIM), k_g), rope_cos, rope_sin)
    k_c = rms_norm(k_c.reshape(b, nc, N_KV_HEADS, HEAD_DIM), k_g)
    v_c = v_c.reshape(b, nc, N_KV_HEADS, HEAD_DIM)
    k_all = jnp.concatenate([k_c, k_l], axis=1)
    v_all = jnp.concatenate([v_c, v_l.reshape(b, n, N_KV_HEADS, HEAD_DIM)], axis=1)
    attn_l = latent_attention(q_l, k_all, v_all)
    conv_l = gb_l * short_conv3(gc_l * u_l, conv_w)
    y_lat = jnp.concatenate([attn_l, conv_l], axis=-1) @ w_out
    if not ctx_live:
        return y_lat, None
    q_c = rms_norm(q_c.reshape(b, nc, N_HEADS, HEAD_DIM), q_g)
    attn_c = attend(q_c.reshape(b, nc, N_KV_HEADS, Q_GROUP, HEAD_DIM), k_c, v_c).reshape(b, nc, ATTN_W)
    conv_c = gb_c * short_conv3(gc_c * u_c, conv_w)
    y_ctx = jnp.concatenate([attn_c, conv_c], axis=-1) @ w_out
    return y_lat, y_ctx


def multiscale_pool(h, pool_w, pool_scale):
    b, n, d = h.shape
    hf = h.astype(jnp.float32)
    cs = jnp.concatenate([jnp.zeros((b, 1, d), jnp.float32), jnp.cumsum(hf, axis=1)], axis=1)
    t = jnp.arange(n)
    outs = []
    for gi, w in enumerate(POOL_WINDOWS):
        lo = jnp.clip(t - w // 2, 0, n)
        hi = jnp.clip(t + w - w // 2, 0, n)
        sl = slice(gi * POOL_GROUP_W, (gi + 1) * POOL_GROUP_W)
        csg = cs[..., sl]
        win_sum = jnp.take(csg, hi, axis=1) - jnp.take(csg, lo, axis=1)
        cnt = (hi - lo).astype(jnp.float32)[None, :, None]
        outs.append(win_sum / cnt - hf[..., sl])
    p = jnp.stack(outs, axis=2).astype(h.dtype)
    y = jnp.einsum('bngc,gce->bnge', p, pool_w).reshape(b, n, d)
    return y * pool_scale


def swiglu(h, wg, wu, wd):
    return (jax.nn.silu(h @ wg) * (h @ wu)) @ wd


def moe_top2(h, router_w, router_b, wg, wu, wd):
    logits = (h @ router_w).astype(jnp.float32) + router_b.astype(jnp.float32)
    top_v, top_i = lax.top_k(logits, TOP_K)
    top_p = jax.nn.softmax(top_v, axis=-1)
    gates = jnp.sum(jax.nn.one_hot(top_i, N_EXPERTS, dtype=jnp.float32) * top_p[..., None], axis=-2)
    out = jnp.zeros_like(h)
    for e in range(N_EXPERTS):
        out = out + gates[..., e:e + 1].astype(h.dtype) * swiglu(h, wg[e], wu[e], wd[e])
    return out


def setup_inputs(seed: int = 0) -> dict:
    key = jax.random.key(seed)
    ks = jax.random.split(key, 24)
    f32 = jnp.float32

    def nrm(k, shape, scale):
        return jax.random.normal(k, shape, f32) * scale

    return {
        "x": nrm(ks[0], (BATCH, SEQ, D_MODEL), 1.0),
        "c": nrm(ks[1], (BATCH, D_MODEL), 1.0),
        "ctx": nrm(ks[2], (BATCH, CTX_LEN, D_MODEL), 1.0),
        "c_ctx": nrm(ks[3], (D_MODEL,), 1.0),
        "w_mod": nrm(ks[4], (DEPTH, D_MODEL, N_MOD * D_MODEL), 0.5 * D_MODEL ** -0.5),
        "b_mod": nrm(ks[5], (DEPTH, N_MOD * D_MODEL), 0.02),
        "norm_g": 1.0 + nrm(ks[6], (DEPTH, 2, D_MODEL), 0.05),
        "final_norm_g": 1.0 + nrm(ks[7], (D_MODEL,), 0.05),
        "w_mix_in": nrm(ks[8], (N_EVEN, D_MODEL, MIX_IN_W), D_MODEL ** -0.5),
        "q_norm_g": 1.0 + nrm(ks[9], (N_EVEN, HEAD_DIM), 0.05),
        "k_norm_g": 1.0 + nrm(ks[10], (N_EVEN, HEAD_DIM), 0.05),
        "conv_w": nrm(ks[11], (N_EVEN, 3, CONV_W), 3 ** -0.5),
        "w_mix_out": nrm(ks[12], (N_EVEN, MIX_OUT_IN, D_MODEL), MIX_OUT_IN ** -0.5),
        "ffn_w_gate": nrm(ks[13], (N_EVEN, D_MODEL, D_FF), D_MODEL ** -0.5),
        "ffn_w_up": nrm(ks[14], (N_EVEN, D_MODEL, D_FF), D_MODEL ** -0.5),
        "ffn_w_down": nrm(ks[15], (N_EVEN, D_FF, D_MODEL), D_FF ** -0.5),
        "pool_w": nrm(ks[16], (N_ODD, N_POOL_GROUPS, POOL_GROUP_W, POOL_GROUP_W), POOL_GROUP_W ** -0.5),
        "pool_scale": 1.0 + nrm(ks[17], (N_ODD, D_MODEL), 0.05),
        "router_w": nrm(ks[18], (N_ODD, D_MODEL, N_EXPERTS), D_MODEL ** -0.5),
        "router_b": nrm(ks[19], (N_ODD, N_EXPERTS), 0.01),
        "exp_w_gate": nrm(ks[20], (N_ODD, N_EXPERTS, D_MODEL, D_FF_EXPERT), D_MODEL ** -0.5),
        "exp_w_up": nrm(ks[21], (N_ODD, N_EXPERTS, D_MODEL, D_FF_EXPERT), D_MODEL ** -0.5),
        "exp_w_down": nrm(ks[22], (N_ODD, N_EXPERTS, D_FF_EXPERT, D_MODEL), D_FF_EXPERT ** -0.5),
    }


def reference(x, c, ctx, c_ctx, w_mod, b_mod, norm_g, final_norm_g, w_mix_in, q_norm_g, k_norm_g,
              conv_w, w_mix_out, ffn_w_gate, ffn_w_up, ffn_w_down, pool_w, pool_scale,
              router_w, router_b, exp_w_gate, exp_w_up, exp_w_down):
    n = x.shape[1]
    rope_cos, rope_sin = axial_rope_tables(n)
    x_lat, x_ctx = x, ctx
    for i in range(DEPTH):
        ctx_live = i < DEPTH - 1
        is_even = (i % 2) == 0
        j = i // 2
        sh1, sc1, g1, sh2, sc2, g2 = [m[:, None, :] for m in adaln(c, w_mod[i], b_mod[i])]
        a_lat = modulate(rms_norm(x_lat, norm_g[i, 0]), sh1, sc1)
        need_ctx_in = ctx_live or is_even
        if need_ctx_in:
            csh1, csc1, cg1, csh2, csc2, cg2 = adaln(c_ctx, w_mod[i], b_mod[i])
            a_ctx = modulate(rms_norm(x_ctx, norm_g[i, 0]), csh1, csc1)
        if is_even:
            y_lat, y_ctx = attn_conv_mixer(a_lat, a_ctx, w_mix_in[j], q_norm_g[j], k_norm_g[j], conv_w[j],
                                           w_mix_out[j], rope_cos, rope_sin, ctx_live)
        else:
            y_lat = multiscale_pool(a_lat, pool_w[j], pool_scale[j])
            y_ctx = multiscale_pool(a_ctx, pool_w[j], pool_scale[j]) if ctx_live else None
        x_lat = x_lat + g1 * y_lat
        f_lat = modulate(rms_norm(x_lat, norm_g[i, 1]), sh2, sc2)
        if ctx_live:
            x_ctx = x_ctx + cg1 * y_ctx
            f_ctx = modulate(rms_norm(x_ctx, norm_g[i, 1]), csh2, csc2)
        if is_even:
            x_lat = x_lat + g2 * swiglu(f_lat, ffn_w_gate[j], ffn_w_up[j], ffn_w_down[j])
            if ctx_live:
                x_ctx = x_ctx + cg2 * swiglu(f_ctx, ffn_w_gate[j], ffn_w_up[j], ffn_w_down[j])
        else:
            x_lat = x_lat + g2 * moe_top2(f_lat, router_w[j], router_b[j], exp_w_gate[j], exp_w_up[j], exp_w_down[j])
            if ctx_live:
                x_ctx = x_ctx + cg2 * moe_top2(f_ctx, router_w[j], router_b[j], exp_w_gate[j], exp_w_up[j], exp_w_down[j])
    return rms_norm(x_lat, final_norm_g)
```

```python
import contextlib
import os
import numpy as np
import concourse.bass as bass
import concourse.mybir as mybir
from concourse.bass_utils import run_bass_kernel_spmd

F32 = mybir.dt.float32
BF16 = mybir.dt.bfloat16
ALU = mybir.AluOpType
AF = mybir.ActivationFunctionType
AX = mybir.AxisListType

D = 1024
SEQ = 8192
CTX = 256
NKEY = SEQ + CTX
NKB = NKEY // 128
DFF = 2816
NE = 8
DFE = 1408
EPS = 1e-6
GT = 512
NG = SEQ // GT
NCORES = 8


class Buf:
    def __init__(self, name, t=None):
        self.name = name
        self.t = t
        self.wev = None
        self.revs = {}
        self.dsem = None
        self.dcnt = 0


class Ctx:
    def __init__(self, nc, es):
        self.nc = nc
        self.es = es
        self.E = {"pe": nc.tensor, "act": nc.scalar, "dve": nc.vector, "pool": nc.gpsimd, "sp": nc.sync}
        self.sem = {}
        self.cnt = {}
        self.waited = {}
        for e in self.E:
            self.sem[e] = es.enter_context(nc.semaphore("eng_" + e))
            self.cnt[e] = 0
            self.waited[e] = {}
        self.nsem = 0
        self.warstat = {}
        self._cur_w = None
        self.dbufs = []
        self.semname = {}
        for e in self.E:
            self.semname[id(self.sem[e])] = e

    def _key(self, sem):
        return id(sem)

    def _wait(self, e, ev):
        sem, val = ev
        k = id(sem)
        if self.waited[e].get(k, 0) >= val:
            return
        en = self.semname.get(k)
        if os.environ.get("KDBG2") and e == "act":
            print("ACT wait on", en or "dma", val, "| act cnt", self.cnt["act"])
        if en is not None:
            lim = self.cnt[en] + (1 if en == "pe" else 0)
            assert val <= lim, ("unreachable wait", e, en, val, lim)
            if en == e and os.environ.get("KDBG"):
                print("self-wait", e, val, self.cnt[en])
        self.E[e].wait_ge(sem, val)
        self.waited[e][k] = val

    def _deps(self, e, reads, writes):
        own = id(self.sem[e]) if e in self.sem else None
        need = {}

        def add(ev, kind):
            if ev is None:
                return
            sem, val = ev
            k = id(sem)
            if k == own:
                if e == "pe":
                    return
                if kind == "war" and os.environ.get("WARSTAT") is not None and self.waited[e].get(k, 0) < val:
                    key = (e, self._cur_w)
                    self.warstat[key] = self.warstat.get(key, 0) + 1
            if need.get(k, (None, 0))[1] < val:
                need[k] = (sem, val)

        for b in reads:
            add(b.wev, "raw")
        for b in writes:
            add(b.wev, "waw")
            self._cur_w = b.name
            for ev in b.revs.values():
                add(ev, "war")
        for ev in need.values():
            self._wait(e, ev)

    def _record(self, ev, reads, writes):
        k = id(ev[0])
        for b in writes:
            b.wev = ev
            b.revs = {}
        for b in reads:
            old = b.revs.get(k)
            if old is None or old[1] < ev[1]:
                b.revs[k] = ev

    def op(self, e, fn, reads=(), writes=(), inc=True):
        self._deps(e, reads, writes)
        ins = fn()
        if inc:
            self.cnt[e] += 1
            ins.then_inc(self.sem[e], 1)
            ev = (self.sem[e], self.cnt[e])
        else:
            ev = (self.sem[e], self.cnt[e] + 1)
        self._record(ev, reads, writes)
        return ins

    def dma(self, q, out, in_, reads=(), writes=(), **kw):
        self._deps(q, reads, writes)
        w = writes[0]
        if w.dsem is None:
            w.dsem = self.es.enter_context(self.nc.semaphore("d_" + w.name))
            self.nsem += 1
            self.dbufs.append(w)
        ins = self.E[q].dma_start(out=out, in_=in_, **kw)
        w.dcnt += 16
        ins.then_inc(w.dsem, 16)
        ev = (w.dsem, w.dcnt)
        self._record(ev, reads, writes)
        return ins

    def barrier(self):
        evs = [(self.sem[e], self.cnt[e]) for e in self.E if self.cnt[e] > 0]
        evs += [(bf.dsem, bf.dcnt) for bf in self.dbufs]
        for e in self.E:
            for ev in evs:
                self._wait(e, ev)

    def wait_all(self, e, bufs):
        for b in bufs:
            if b.wev is not None:
                self._wait(e, b.wev)


def build(debug=False, stop_after=None):
    nc = bass.Bass("TRN2", target_bir_lowering=False)

    def din(name, shape, dt=F32):
        return nc.dram_tensor(name, list(shape), dt, kind="ExternalInput").ap()

    x_d = din("x", [SEQ, D])
    c_d = din("c", [D])
    ctx_d = din("ctx", [CTX, D])
    cctx_d = din("c_ctx", [D])
    wmod_d = din("w_mod", [2, D, 6 * D])
    bmod_d = din("b_mod", [2, 6 * D])
    ng_d = din("norm_g", [2, 2, D])
    fg_d = din("final_norm_g", [D])
    wmi_d = din("w_mix_in", [D, 2304])
    qg_d = din("q_norm_g", [64])
    kg_d = din("k_norm_g", [64])
    cw_d = din("conv_w", [3, 512])
    wmo_d = din("w_mix_out", [D, D])
    wg_d = din("ffn_w_gate", [D, DFF])
    wu_d = din("ffn_w_up", [D, DFF])
    wd_d = din("ffn_w_down", [DFF, D])
    pw_d = din("pool_w", [4, 256, 256])
    psc_d = din("pool_scale", [D])
    rw_d = din("router_w", [D, NE])
    rb_d = din("router_b", [NE])
    eg_d = din("exp_w_gate", [NE, D, DFE])
    eu_d = din("exp_w_up", [NE, D, DFE])
    ed_d = din("exp_w_down", [NE, DFE, D])
    ident_d = din("ident", [128, 128])
    cos_d = din("rope_cos", [SEQ, 32])
    sin_d = din("rope_sin", [SEQ, 32])
    icnt_d = din("pool_icnt", [64])

    out_d = nc.dram_tensor("out", [SEQ, D], F32, kind="ExternalOutput").ap()
    skind = "ExternalOutput" if debug else "Internal"
    xmid_d = nc.dram_tensor("xmid_s", [SEQ, D], F32, kind=skind).ap()
    x1_d = nc.dram_tensor("x1_s", [SEQ, D], F32, kind=skind).ap()
    wmi_s = nc.dram_tensor("wmi_s", [128, 8, 2304], BF16, kind="Internal").ap()
    woA_s = nc.dram_tensor("woA_s", [128, 4, D], BF16, kind="Internal").ap()
    woC_s = nc.dram_tensor("woC_s", [128, 4, D], BF16, kind="Internal").ap()
    pwb_s = nc.dram_tensor("pwb_s", [128, 8, 256], BF16, kind="Internal").ap()
    rwb_s = nc.dram_tensor("rwb_s", [128, 8, NE], BF16, kind="Internal").ap()
    wgu_s = nc.dram_tensor("wgu_s", [22, 128, 2 * 8 * 128], BF16, kind="Internal").ap()
    wd_s = nc.dram_tensor("wd_s", [128, 22, D], BF16, kind="Internal").ap()
    egu_s = nc.dram_tensor("egu_s", [NE * 11, 128, 2 * 8 * 128], BF16, kind="Internal").ap()
    ed_s = nc.dram_tensor("ed_s", [NE, 128, 11, D], BF16, kind="Internal").ap()
    if debug:
        dbg_kt = nc.dram_tensor("dbg_kt", [128, NKEY], F32, kind="ExternalOutput").ap()
        dbg_v = nc.dram_tensor("dbg_v", [128, NKB * 192], F32, kind="ExternalOutput").ap()

    with contextlib.ExitStack() as es:
        K = Ctx(nc, es)

        def mk_sb(stack):
            def sb(name, shape, dt=F32):
                t = stack.enter_context(nc.sbuf_tensor("s_" + name, list(shape), dt))
                return Buf(name, t)
            return sb

        sb = mk_sb(es)
        PS = []
        for i in range(8):
            t = es.enter_context(nc.psum_tensor("psb%d" % i, [128, 512], F32))
            PS.append(Buf("psb%d" % i, t))

        xmid_b = Buf("xmid_dram")
        x1_b = Buf("x1_dram")
        wsm_b = Buf("wsmall_dram")
        wgu_b = Buf("wgu_dram")
        wd_b = Buf("wd_dram")
        egu_b = Buf("egu_dram")
        ed_b = Buf("ed_dram")
        out_b = Buf("out_dram")
        dbg_b = Buf("dbg_dram")
        fin = [out_b, xmid_b, x1_b, wsm_b, wgu_b, wd_b, egu_b, ed_b, dbg_b]

        def finish():
            K.barrier()
            print("instr counts", K.cnt, "dma sems", K.nsem)
            if K.warstat:
                print("WARSTAT", sorted(K.warstat.items(), key=lambda kv: -kv[1])[:25])

        ident = sb("ident", [128, 128])
        K.dma("sp", ident.t[:], ident_d[:, :], writes=[ident])
        epsb = sb("epsb", [128, 1])
        K.op("pool", lambda: nc.gpsimd.memset(epsb.t[:], EPS), writes=[epsb])
        gsc = sb("gsc", [128, 5, 8])
        shc = sb("shc", [128, 5, 8])
        cwc = sb("cwc", [128, 3, 4])
        rowt = sb("rowt", [128, 128])

        def load_cols(dst_ap, dst_buf, src2d, n):
            K.dma("sp", rowt.t[0:n, :], src2d, writes=[rowt])
            K.op("pe", lambda: nc.tensor.transpose(PS[7].t[:, 0:n], rowt.t[0:n, :], ident.t[0:n, 0:n]),
                 reads=[rowt, ident], writes=[PS[7]])
            K.op("dve", lambda: nc.vector.tensor_copy(out=dst_ap, in_=PS[7].t[:, 0:n]), reads=[PS[7]], writes=[dst_buf])

        load_cols(cwc.t[:].rearrange("p t k -> p (t k)"), cwc, cw_d.rearrange("t (k p) -> (t k) p", p=128), 12)

        def bc_row(name, src_ap, n, sbf=None):
            b = (sbf or sb)(name, [128, n])
            K.dma("sp", b.t[:], src_ap.partition_broadcast(128), writes=[b])
            return b

        qg_bc = bc_row("qg_bc", qg_d, 64)
        kg_bc = bc_row("kg_bc", kg_d, 64)
        rb_bc = bc_row("rb_bc", rb_d, NE)
        icnt_bc = bc_row("icnt_bc", icnt_d, 64)
        K.op("dve", lambda: nc.vector.tensor_scalar(out=qg_bc.t[:], in0=qg_bc.t[:], scalar1=0.125, scalar2=None,
                                                    op0=ALU.mult), reads=[qg_bc], writes=[qg_bc])

        xin = [sb("xin%d" % i, [128, D]) for i in range(4)]
        xin_i = [0]
        xn = [sb("xn%d" % i, [128, D]) for i in range(4)]
        xn_i = [0]
        junk = sb("junk", [128, D])
        ssq = [sb("ssq%d" % i, [128, 2]) for i in range(8)]
        ssq_i = [0]
        aT = sb("aT", [128, 8, GT], BF16)
        aTk = [[Buf("aT_%d_%d" % (ti, k), aT.t) for k in range(8)] for ti in range(4)]

        def aTr(k, ti=None):
            if ti is None:
                return [aTk[t][k] for t in range(4)]
            return [aTk[ti][k]]

        def next_xin():
            b = xin[xin_i[0] % 4]
            xin_i[0] += 1
            return b

        if stop_after == "consts":
            finish()
            return nc
        with contextlib.ExitStack() as ph:
            sbp = mk_sb(ph)
            ccol = sbp("ccol", [128, 2, 8])
            load_cols(ccol.t[:, 0, :], ccol, c_d.rearrange("(k p) -> k p", p=128), 8)
            load_cols(ccol.t[:, 1, :], ccol, cctx_d.rearrange("(k p) -> k p", p=128), 8)
            scol = sbp("scol", [128, 2, 8])
            K.op("act", lambda: nc.scalar.activation(out=scol.t[:], in_=ccol.t[:], func=AF.Silu),
                 reads=[ccol], writes=[scol])
            scol2 = sbp("scol2", [128, 8, 2])
            K.op("dve", lambda: nc.vector.tensor_copy(out=scol2.t[:], in_=scol.t[:].rearrange("p a k -> p k a")),
                 reads=[scol], writes=[scol2])
            srep = sbp("srep", [128, 8, 128])
            K.op("dve", lambda: nc.vector.tensor_copy(
                out=srep.t[:], in_=scol.t[:, 0, :].unsqueeze(2).to_broadcast([128, 8, 128])),
                reads=[scol], writes=[srep])

            modc = [sbp("modc%d" % l, [128, 6, 8]) for l in range(2)]
            modx = sbp("modx", [128, 2, 8])
            bcol = [sbp("bcol%d" % l, [128, 48]) for l in range(2)]
            ngcol = sbp("ngcol", [128, 4, 8])
            for l in range(2):
                load_cols(bcol[l].t[:], bcol[l], bmod_d[l].rearrange("(k p) -> k p", p=128), 48)
            load_cols(ngcol.t[:].rearrange("p a k -> p (a k)"), ngcol,
                      ng_d.rearrange("l j (k p) -> (l j k) p", p=128), 32)
            grow = [[sbp("grow%d_%d" % (l, j), [128, D]) for j in range(2)] for l in range(2)]
            brow = sbp("brow", [128, D])
            psc_bc = bc_row("psc_bc", psc_d, D, sbp)

            wst = [sbp("wst%d" % i, [128, 8, 512]) for i in range(2)]
            wst_i = [0]

            def stage():
                b = wst[wst_i[0] % 2]
                wst_i[0] += 1
                return b

            cst = [sbp("cst%d" % i, [128, 8, 512], BF16) for i in range(2)]
            cst_i = [0]

            def cstage():
                b = cst[cst_i[0] % 2]
                cst_i[0] += 1
                return b

            for l in range(2):
                for blk in range(12):
                    st = stage()
                    K.dma("sp", st.t[:], wmod_d[l, :, blk * 512:(blk + 1) * 512].rearrange("(k p) n -> p k n", p=128),
                          writes=[st])
                    which = blk // 2
                    if which in (2, 5):
                        j = 0 if which == 2 else 1
                        half = blk % 2
                        pb = PS[blk % 2]
                        off = which * D + half * 512
                        K.dma("sp", brow.t[:, 0:512], bmod_d[l, off:off + 512].partition_broadcast(128), writes=[brow])
                        for k in range(8):
                            K.op("pe", lambda k=k, pb=pb, st=st: nc.tensor.matmul(
                                pb.t[:], lhsT=srep.t[:, k, :], rhs=st.t[:, k, :], start=(k == 0), stop=(k == 7)),
                                reads=[srep, st], writes=[pb], inc=(k == 7))
                        K.op("dve", lambda pb=pb, l=l, j=j, half=half: nc.vector.tensor_tensor(
                            out=grow[l][j].t[:, half * 512:(half + 1) * 512], in0=pb.t[:],
                            in1=brow.t[:, 0:512], op=ALU.add),
                            reads=[pb, brow], writes=[grow[l][j]])
                    else:
                        pb = PS[2 + blk % 2]
                        for cc in range(4):
                            for k in range(8):
                                K.op("pe", lambda k=k, cc=cc, pb=pb, st=st: nc.tensor.matmul(
                                    pb.t[:, cc * 2:cc * 2 + 2], lhsT=st.t[:, k, cc * 128:(cc + 1) * 128],
                                    rhs=scol2.t[:, k, :], start=(k == 0), stop=(k == 7)),
                                    reads=[scol2, st], writes=[pb], inc=(k == 7 and cc == 3))
                        c0 = (blk % 2) * 4
                        pv = pb.t[:, 0:8].rearrange("p (c two) -> p c two", two=2)
                        K.op("dve", lambda pv=pv, l=l, which=which, c0=c0, blk=blk, pb=pb: nc.vector.tensor_tensor(
                            out=modc[l].t[:, which, c0:c0 + 4], in0=pv[:, :, 0],
                            in1=bcol[l].t[:, blk * 4:blk * 4 + 4], op=ALU.add),
                            reads=[pb, bcol[l]], writes=[modc[l]])
                        if l == 0 and which in (0, 1):
                            K.op("dve", lambda pv=pv, which=which, c0=c0, blk=blk, pb=pb: nc.vector.tensor_tensor(
                                out=modx.t[:, which, c0:c0 + 4], in0=pv[:, :, 1],
                                in1=bcol[0].t[:, blk * 4:blk * 4 + 4], op=ALU.add),
                                reads=[pb, bcol[0]], writes=[modx])

            if stop_after == "adaln":
                finish()
                return nc
            for i, (l, j) in enumerate([(0, 0), (0, 1), (1, 0), (1, 1)]):
                K.op("dve", lambda i=i, l=l, j=j: nc.vector.scalar_tensor_tensor(
                    out=gsc.t[:, i, :], in0=modc[l].t[:, 1 + 3 * j, :], scalar=1.0, in1=ngcol.t[:, 2 * l + j, :],
                    op0=ALU.add, op1=ALU.mult), reads=[modc[l], ngcol], writes=[gsc])
                K.op("dve", lambda i=i, l=l, j=j: nc.vector.tensor_copy(out=shc.t[:, i, :], in_=modc[l].t[:, 3 * j, :]),
                     reads=[modc[l]], writes=[shc])
            K.op("dve", lambda: nc.vector.scalar_tensor_tensor(
                out=gsc.t[:, 4, :], in0=modx.t[:, 1, :], scalar=1.0, in1=ngcol.t[:, 0, :],
                op0=ALU.add, op1=ALU.mult), reads=[modx, ngcol], writes=[gsc])
            K.op("dve", lambda: nc.vector.tensor_copy(out=shc.t[:, 4, :], in_=modx.t[:, 0, :]),
                 reads=[modx], writes=[shc])
            K.op("dve", lambda: nc.vector.tensor_tensor(out=psc_bc.t[:], in0=psc_bc.t[:], in1=grow[1][0].t[:], op=ALU.mult),
                 reads=[psc_bc, grow[1][0]], writes=[psc_bc])
            pcs = psc_bc

            cast_rr = [0]

            def cast(out_ap, in_ap, reads, writes, mul_ap=None, mul_buf=None):
                e = ("dve", "pool", "act")[cast_rr[0] % 3]
                cast_rr[0] += 1
                if mul_ap is not None:
                    if e == "act":
                        e = "dve"
                    eng = nc.vector if e == "dve" else nc.gpsimd
                    K.op(e, lambda: eng.tensor_tensor(out=out_ap, in0=in_ap, in1=mul_ap, op=ALU.mult),
                         reads=list(reads) + [mul_buf], writes=writes)
                elif e == "act":
                    K.op(e, lambda: nc.scalar.copy(out=out_ap, in_=in_ap), reads=reads, writes=writes)
                else:
                    eng = nc.vector if e == "dve" else nc.gpsimd
                    K.op(e, lambda: eng.tensor_copy(out=out_ap, in_=in_ap), reads=reads, writes=writes)

            wmi_src = wmi_d.rearrange("(k p) n -> p k n", p=128)
            st = stage()
            cs = cstage()
            qsrc = wmi_d[:, 0:512].rearrange("(k p) (hi j d) -> p k hi j d", p=128, hi=2, j=4)
            stv = st.t[:].rearrange("p k (j hi d) -> p k j hi d", j=4, hi=2)
            for hi in range(2):
                for j in range(4):
                    K.dma("sp", stv[:, :, j, hi, :], qsrc[:, :, hi, j, :], writes=[st])
            cast(cs.t[:], st.t[:], [st], [cs])
            K.dma("pool", wmi_s[:, :, 0:512], cs.t[:], reads=[cs], writes=[wsm_b])
            for blk in range(1, 5):
                n0 = blk * 512
                n1 = min(2304, n0 + 512)
                st = stage()
                cs = cstage()
                K.dma("sp", st.t[:, :, 0:n1 - n0], wmi_src[:, :, n0:n1], writes=[st])
                cast(cs.t[:, :, 0:n1 - n0], st.t[:, :, 0:n1 - n0], [st], [cs])
                K.dma("pool", wmi_s[:, :, n0:n1], cs.t[:, :, 0:n1 - n0], reads=[cs], writes=[wsm_b])
            g1r = grow[0][0]
            for half in range(2):
                st = stage()
                cs = cstage()
                for j in range(4):
                    for hi in range(2):
                        h = j + 4 * hi
                        K.dma("sp", st.t[hi * 64:(hi + 1) * 64, j, :],
                              wmo_d[h * 64:(h + 1) * 64, half * 512:(half + 1) * 512], writes=[st])
                K.dma("sp", st.t[:, 4:8, :],
                      wmo_d[512:1024, half * 512:(half + 1) * 512].rearrange("(k p) n -> p k n", p=128), writes=[st])
                g1v = g1r.t[:, half * 512:(half + 1) * 512].unsqueeze(1).to_broadcast([128, 8, 512])
                cast(cs.t[:], st.t[:], [st], [cs], mul_ap=g1v, mul_buf=g1r)
                K.dma("pool", woA_s[:, :, half * 512:(half + 1) * 512], cs.t[:, 0:4, :], reads=[cs], writes=[wsm_b])
                K.dma("pool", woC_s[:, :, half * 512:(half + 1) * 512], cs.t[:, 4:8, :], reads=[cs], writes=[wsm_b])
            st = stage()
            cs = cstage()
            K.dma("sp", st.t[:, :, 0:256], pw_d.rearrange("g (kk p) e -> p (g kk) e", p=128), writes=[st])
            for gi in range(4):
                pv = pcs.t[:, gi * 256:(gi + 1) * 256].unsqueeze(1).to_broadcast([128, 2, 256])
                cast(cs.t[:, 2 * gi:2 * gi + 2, 0:256], st.t[:, 2 * gi:2 * gi + 2, 0:256], [st], [cs], mul_ap=pv, mul_buf=pcs)
            K.dma("pool", pwb_s[:, :, :], cs.t[:, :, 0:256], reads=[cs], writes=[wsm_b])
            st = stage()
            cs = cstage()
            K.dma("sp", st.t[:, :, 0:NE], rw_d.rearrange("(k p) e -> p k e", p=128), writes=[st])
            cast(cs.t[:, :, 0:NE], st.t[:, :, 0:NE], [st], [cs])
            K.dma("pool", rwb_s[:, :, :], cs.t[:, :, 0:NE], reads=[cs], writes=[wsm_b])

            if stop_after == "small":
                finish()
                return nc

            def prep_gu(g_src, u_src, dst_ap, dst_buf, jn):
                for j0 in range(0, jn, 2):
                    nj = min(2, jn - j0)
                    st = stage()
                    cs = cstage()
                    stf = st.t[:].rearrange("p k n -> p (k n)")
                    csf = cs.t[:].rearrange("p k n -> p (k n)")
                    st4 = stf.rearrange("p (jm k n) -> p jm k n", k=8, n=128)
                    for j in range(nj):
                        for m, src in enumerate((g_src, u_src)):
                            K.dma("sp", st4[:, j * 2 + m, :, :],
                                  src[:, (j0 + j) * 128:(j0 + j + 1) * 128].rearrange("(k p) n -> p k n", p=128),
                                  writes=[st])
                    cast(csf[:, 0:nj * 2048], stf[:, 0:nj * 2048], [st], [cs])
                    for j in range(nj):
                        K.dma("pool", dst_ap[j0 + j], csf[:, j * 2048:(j + 1) * 2048], reads=[cs], writes=[dst_buf])

            def prep_d(d_src, dst_ap, dst_buf, jn, grow_b):
                for j0 in range(0, jn, 4):
                    nj = min(4, jn - j0)
                    st = stage()
                    cs = cstage()
                    stv = st.t[:].rearrange("p k n -> p (k n)").rearrange("p (j n) -> p j n", n=D)
                    csv = cs.t[:].rearrange("p k n -> p (k n)").rearrange("p (j n) -> p j n", n=D)
                    K.dma("sp", stv[:, 0:nj, :], d_src[j0 * 128:(j0 + nj) * 128, :].rearrange("(j p) n -> p j n", p=128),
                          writes=[st])
                    gv = grow_b.t[:].unsqueeze(1).to_broadcast([128, nj, D])
                    cast(csv[:, 0:nj, :], stv[:, 0:nj, :], [st], [cs], mul_ap=gv, mul_buf=grow_b)
                    K.dma("pool", dst_ap[:, j0:j0 + nj, :], csv[:, 0:nj, :], reads=[cs], writes=[dst_buf])

            prep_gu(wg_d, wu_d, wgu_s, wgu_b, 22)
            if stop_after == "gu1":
                finish()
                return nc
            prep_d(wd_d, wd_s, wd_b, 22, grow[0][1])
            if stop_after == "d1":
                finish()
                return nc
            for e in range(NE):
                prep_gu(eg_d[e], eu_d[e], egu_s[e * 11:(e + 1) * 11], egu_b, 11)
                prep_d(ed_d[e], ed_s[e], ed_b, 11, grow[1][1])
            K.barrier()
        if stop_after == "setup":
            finish()
            return nc

        def rms_rstd_multi(items):
            ss = []
            for _ in items:
                s_ = ssq[ssq_i[0] % 8]
                ssq_i[0] += 1
                ss.append(s_)
                K.op("pool", lambda s_=s_: nc.gpsimd.memset(s_.t[:], 0.0), writes=[s_])
            for (x_ap, x_buf), s_ in zip(items, ss):
                K.op("act", lambda x_ap=x_ap, s_=s_: nc.scalar.activation(out=junk.t[:], in_=x_ap, func=AF.Square,
                                                                        accum_out=s_.t[:, 0:1]),
                     reads=(x_buf if isinstance(x_buf, list) else [x_buf]) + [s_], writes=[junk, s_])
            for s_ in ss:
                K.op("act", lambda s_=s_: nc.scalar.activation(out=s_.t[:, 1:2], in_=s_.t[:, 0:1], func=AF.Sqrt,
                                                               bias=epsb.t[:], scale=1.0 / D),
                     reads=[s_, epsb], writes=[s_])
            for s_ in ss:
                K.op("dve", lambda s_=s_: nc.vector.reciprocal(out=s_.t[:, 1:2], in_=s_.t[:, 1:2]), reads=[s_], writes=[s_])
            return ss

        def rms_rstd(x_ap, x_buf):
            return rms_rstd_multi([(x_ap, x_buf)])[0]

        tr_rr = [0]

        def norm_transpose_multi(items):
            if len(items) > 4:
                norm_transpose_multi(items[:4])
                norm_transpose_multi(items[4:])
                return
            ss = rms_rstd_multi([(it[0], it[1]) for it in items])
            xbs = []
            for (x_ap, x_buf, mi, evac, dbf), s_ in zip(items, ss):
                xb = xn[xn_i[0] % 4]
                xn_i[0] += 1
                xbs.append(xb)
                K.op("act", lambda xb=xb, x_ap=x_ap, s_=s_: nc.scalar.activation(out=xb.t[:], in_=x_ap, func=AF.Identity,
                                                                                scale=s_.t[:, 1:2]),
                     reads=(x_buf if isinstance(x_buf, list) else [x_buf]) + [s_], writes=[xb])
            for (x_ap, x_buf, mi, evac, dbf), xb in zip(items, xbs):
                trb = (PS[0], PS[1]) if tr_rr[0] % 2 == 0 else (PS[2], PS[3])
                tr_rr[0] += 1
                for half in range(2):
                    pb = trb[half]
                    for kk in range(4):
                        k = half * 4 + kk
                        K.op("pe", lambda k=k, kk=kk, pb=pb, xb=xb: nc.tensor.transpose(
                            pb.t[:, kk * 128:(kk + 1) * 128], xb.t[:, k * 128:(k + 1) * 128], ident.t[:]),
                            reads=[xb, ident], writes=[pb], inc=(kk == 3))
                    for kk in range(4):
                        k = half * 4 + kk
                        evac(k, pb.t[:, kk * 128:(kk + 1) * 128], gsc.t[:, mi, k:k + 1], shc.t[:, mi, k:k + 1],
                             "dve" if half == 0 else "act", [pb, gsc, shc], [dbf(k)])

        def norm_transpose(x_ap, x_buf, mi, evac, dbf, trb=None):
            norm_transpose_multi([(x_ap, x_buf, mi, evac, dbf)])

        def std_evac(dst_fn):
            def ev(k, p_ap, sc_ap, b_ap, eng, reads, writes):
                if eng == "dve":
                    K.op("dve", lambda: nc.vector.tensor_scalar(out=dst_fn(k), in0=p_ap, scalar1=sc_ap, scalar2=b_ap,
                                                                op0=ALU.mult, op1=ALU.add), reads=reads, writes=writes)
                else:
                    K.op("act", lambda: nc.scalar.activation(out=dst_fn(k), in_=p_ap, func=AF.Identity,
                                                             scale=sc_ap, bias=b_ap), reads=reads, writes=writes)
            return ev

        HS_ = [dict(hq=sb("hq%d" % i, [128, 512]), hsq=sb("hsq%d" % i, [128, 512]), hss=sb("hss%d" % i, [128, 2, 8]),
                    hro=sb("hro%d" % i, [128, 512]), hr1=sb("hr1%d" % i, [128, 256]), hr2=sb("hr2%d" % i, [128, 256]))
               for i in range(2)]
        ropc = sb("ropc", [128, 4, 32])
        rops = sb("rops", [128, 4, 32])

        def head_norm_rope_multi(items):
            assert len(items) <= 2
            T = [HS_[i] for i in range(len(items))]
            its = list(zip(items, T))
            for (pb, nh, g_bc, rt), t in its:
                n = nh * 64
                K.op("dve", lambda pb=pb, t=t, n=n: nc.vector.tensor_copy(out=t["hq"].t[:, 0:n], in_=pb.t[:, 0:n]),
                     reads=[pb], writes=[t["hq"]])
            for (pb, nh, g_bc, rt), t in its:
                n = nh * 64
                K.op("dve", lambda t=t, n=n: nc.vector.tensor_tensor(out=t["hsq"].t[:, 0:n], in0=t["hq"].t[:, 0:n],
                                                                     in1=t["hq"].t[:, 0:n], op=ALU.mult),
                     reads=[t["hq"]], writes=[t["hsq"]])
            for (pb, nh, g_bc, rt), t in its:
                n = nh * 64
                K.op("dve", lambda t=t, n=n, nh=nh: nc.vector.tensor_reduce(
                    out=t["hss"].t[:, 0, 0:nh], in_=t["hsq"].t[:, 0:n].rearrange("p (h d) -> p h d", d=64), axis=AX.X, op=ALU.add),
                    reads=[t["hsq"]], writes=[t["hss"]])
            for (pb, nh, g_bc, rt), t in its:
                K.op("act", lambda t=t, nh=nh: nc.scalar.activation(out=t["hss"].t[:, 1, 0:nh], in_=t["hss"].t[:, 0, 0:nh],
                                                                    func=AF.Sqrt, bias=epsb.t[:], scale=1.0 / 64),
                     reads=[t["hss"], epsb], writes=[t["hss"]])
            for (pb, nh, g_bc, rt), t in its:
                K.op("dve", lambda t=t, nh=nh: nc.vector.reciprocal(out=t["hss"].t[:, 1, 0:nh], in_=t["hss"].t[:, 1, 0:nh]),
                     reads=[t["hss"]], writes=[t["hss"]])
            for (pb, nh, g_bc, rt), t in its:
                n = nh * 64
                hv = t["hq"].t[:, 0:n].rearrange("p (h d) -> p h d", d=64)
                K.op("dve", lambda t=t, nh=nh, hv=hv: nc.vector.tensor_tensor(
                    out=hv, in0=hv, in1=t["hss"].t[:, 1, 0:nh].unsqueeze(2).to_broadcast([128, nh, 64]), op=ALU.mult),
                    reads=[t["hq"], t["hss"]], writes=[t["hq"]])
            for (pb, nh, g_bc, rt), t in its:
                n = nh * 64
                hv = t["hq"].t[:, 0:n].rearrange("p (h d) -> p h d", d=64)
                dst = t["hro"] if rt is None else t["hq"]
                K.op("pool", lambda t=t, nh=nh, hv=hv, dst=dst, n=n, g_bc=g_bc: nc.gpsimd.tensor_tensor(
                    out=dst.t[:, 0:n].rearrange("p (h d) -> p h d", d=64), in0=hv,
                    in1=g_bc.t[:].unsqueeze(1).to_broadcast([128, nh, 64]), op=ALU.mult),
                    reads=[t["hq"], g_bc], writes=[dst])
            rp = [((pb, nh, g_bc, rt), t) for (pb, nh, g_bc, rt), t in its if rt is not None]

            def views(nh, rt, t):
                n = nh * 64
                q5 = t["hq"].t[:, 0:n].rearrange("p (h a t j) -> p h a t j", a=2, t=2, j=16)
                o5 = t["hro"].t[:, 0:n].rearrange("p (h a t j) -> p h a t j", a=2, t=2, j=16)
                cb = ropc.t[:, rt, :].rearrange("p (a j) -> p a j", a=2).unsqueeze(1).to_broadcast([128, nh, 2, 16])
                sv = rops.t[:, rt, :].rearrange("p (a j) -> p a j", a=2).unsqueeze(1).to_broadcast([128, nh, 2, 16])
                m = nh * 32
                t1 = t["hr1"].t[:, 0:m].rearrange("p (h a j) -> p h a j", a=2, j=16)
                t2 = t["hr2"].t[:, 0:m].rearrange("p (h a j) -> p h a j", a=2, j=16)
                return q5[:, :, :, 0, :], q5[:, :, :, 1, :], o5[:, :, :, 0, :], o5[:, :, :, 1, :], cb, sv, t1, t2

            for (pb, nh, g_bc, rt), t in rp:
                x1v, x2v, o1, o2, cb, sv, t1, t2 = views(nh, rt, t)
                K.op("dve", lambda t1=t1, x1v=x1v, cb=cb: nc.vector.tensor_tensor(out=t1, in0=x1v, in1=cb, op=ALU.mult),
                     reads=[t["hq"], ropc], writes=[t["hr1"]])
                K.op("pool", lambda t2=t2, x2v=x2v, sv=sv: nc.gpsimd.tensor_tensor(out=t2, in0=x2v, in1=sv, op=ALU.mult),
                     reads=[t["hq"], rops], writes=[t["hr2"]])
            for (pb, nh, g_bc, rt), t in rp:
                x1v, x2v, o1, o2, cb, sv, t1, t2 = views(nh, rt, t)
                K.op("dve", lambda o1=o1, t1=t1, t2=t2: nc.vector.tensor_tensor(out=o1, in0=t1, in1=t2, op=ALU.subtract),
                     reads=[t["hr1"], t["hr2"]], writes=[t["hro"]])
            for (pb, nh, g_bc, rt), t in rp:
                x1v, x2v, o1, o2, cb, sv, t1, t2 = views(nh, rt, t)
                K.op("dve", lambda t1=t1, x1v=x1v, sv=sv: nc.vector.tensor_tensor(out=t1, in0=x1v, in1=sv, op=ALU.mult),
                     reads=[t["hq"], rops], writes=[t["hr1"]])
                K.op("pool", lambda t2=t2, x2v=x2v, cb=cb: nc.gpsimd.tensor_tensor(out=t2, in0=x2v, in1=cb, op=ALU.mult),
                     reads=[t["hq"], ropc], writes=[t["hr2"]])
            for (pb, nh, g_bc, rt), t in rp:
                x1v, x2v, o1, o2, cb, sv, t1, t2 = views(nh, rt, t)
                K.op("dve", lambda o2=o2, t1=t1, t2=t2: nc.vector.tensor_tensor(out=o2, in0=t1, in1=t2, op=ALU.add),
                     reads=[t["hr1"], t["hr2"]], writes=[t["hro"]])
            return [t["hro"] for t in T]

        def load_rope(t0):
            K.dma("sp", ropc.t[:], cos_d[t0:t0 + 512, :].rearrange("(t p) n -> p t n", p=128), writes=[ropc])
            K.dma("sp", rops.t[:], sin_d[t0:t0 + 512, :].rearrange("(t p) n -> p t n", p=128), writes=[rops])

        with contextlib.ExitStack() as ph:
            sbp = mk_sb(ph)
            wmi = sbp("wmi", [128, 8, 2304], BF16)
            K.dma("sp", wmi.t[:], wmi_s[:, :, :], reads=[wsm_b], writes=[wmi])
            KT = sbp("KT", [128, NKEY], BF16)
            VX = sbp("VX", [128, NKB, 192], BF16)
            K.op("pool", lambda: nc.gpsimd.memset(VX.t[:, :, 64:128], 1.0), writes=[VX])

            for kp in range(NKB // 2):
                kts = (2 * kp, 2 * kp + 1)
                is_ctx = kp == 0
                xbs = []
                for i, kt in enumerate(kts):
                    xb = next_xin()
                    xbs.append(xb)
                    if is_ctx:
                        K.dma("sp", xb.t[:], ctx_d[kt * 128:(kt + 1) * 128, :], writes=[xb])
                    else:
                        t0 = (kt - 2) * 128
                        K.dma("sp", xb.t[:], x_d[t0:t0 + 128, :], writes=[xb])
                        if (kt - 2) % 4 == 0:
                            load_rope(t0)
                norm_transpose_multi([(xbs[i].t[:], xbs[i], 4 if is_ctx else 0,
                                       std_evac(lambda k, i=i: aT.t[:, k, i * 128:(i + 1) * 128]),
                                       lambda k, i=i: aTk[i][k]) for i in range(2)])
                for i, kt in enumerate(kts):
                    pk = PS[4 + i]
                    for k in range(8):
                        K.op("pe", lambda k=k, pk=pk, i=i: nc.tensor.matmul(
                            pk.t[:, 0:256], lhsT=aT.t[:, k, i * 128:(i + 1) * 128], rhs=wmi.t[:, k, 512:768],
                            start=(k == 0), stop=(k == 7)), reads=aTr(k, i) + [wmi], writes=[pk], inc=(k == 7))
                    K.op("dve", lambda pk=pk, kt=kt: nc.vector.tensor_copy(
                        out=VX.t[:, kt, :].rearrange("p (a n) -> p a n", n=64)[:, 0:3:2, :],
                        in_=pk.t[:, 128:256].rearrange("p (a n) -> p a n", n=64)), reads=[pk], writes=[VX])
                hros = head_norm_rope_multi([(PS[4 + i], 2, kg_bc, None if is_ctx else (kt - 2) % 4)
                                             for i, kt in enumerate(kts)])
                for i, kt in enumerate(kts):
                    pt = PS[6 + i]
                    K.op("pe", lambda pt=pt, i=i: nc.tensor.transpose(pt.t[:, 0:128], hros[i].t[:, 0:128], ident.t[:]),
                         reads=[hros[i], ident], writes=[pt])
                    K.op("act", lambda pt=pt, kt=kt: nc.scalar.copy(out=KT.t[:, kt * 128:(kt + 1) * 128], in_=pt.t[:, 0:128]),
                         reads=[pt], writes=[KT])

            if debug and not os.environ.get("DBG_NOKV"):
                with contextlib.ExitStack() as dph:
                    sbd = mk_sb(dph)
                    dbt = sbd("dbt", [128, NKEY])
                    K.op("dve", lambda: nc.vector.tensor_copy(out=dbt.t[:], in_=KT.t[:]), reads=[KT], writes=[dbt])
                    K.dma("pool", dbg_kt[:, :], dbt.t[:], reads=[dbt], writes=[dbg_b])
                    dbv = sbd("dbv", [128, NKB * 192])
                    K.op("dve", lambda: nc.vector.tensor_copy(out=dbv.t[:], in_=VX.t[:].rearrange("p a b -> p (a b)")),
                         reads=[VX], writes=[dbv])
                    K.dma("pool", dbg_v[:, :], dbv.t[:], reads=[dbv], writes=[dbg_b])
                    K.barrier()
            if stop_after == "kv":
                finish()
                return nc

            woA = sbp("woA", [128, 4, D], BF16)
            woC = sbp("woC", [128, 4, D], BF16)
            K.dma("sp", woA.t[:], woA_s[:, :, :], reads=[wsm_b], writes=[woA])
            K.dma("sp", woC.t[:], woC_s[:, :, :], reads=[wsm_b], writes=[woC])
            qT = sbp("qT", [128, 8, GT], BF16)
            K.op("pool", lambda: nc.gpsimd.memset(qT.t[64:128, 0:4, :], 0.0), writes=[qT])
            K.op("pool", lambda: nc.gpsimd.memset(qT.t[0:64, 4:8, :], 0.0), writes=[qT])
            vext = sbp("vext", [128, GT + 2])
            gcs = sbp("gcs", [128, GT])
            gbs = sbp("gbs", [128, GT])
            cacc = sbp("cacc", [128, GT])
            convT = sbp("convT", [128, 4, GT], BF16)
            attnT = sbp("attnT", [128, 4, GT], BF16)
            NPT = 4
            PT = [sbp("PT%d" % i, [128, GT], BF16) for i in range(NPT)]
            dsh = [sbp("dsh%d" % i, [128, GT]) for i in range(2)]
            xo = [sbp("xoa%d" % i, [128, D]) for i in range(2)]
            xo_i = [0]
            vb = sbp("vb", [128, 4, 32])
            print("SBUF remaining after phase A alloc:", nc.sbuf_bytes_remaining)

            xb = next_xin()
            K.op("pool", lambda: nc.gpsimd.memset(xb.t[:], 0.0), writes=[xb])
            for g in range(NG - 1):
                K.dma("sp", xb.t[2 * g:2 * g + 2, :], x_d[512 * g + 511:512 * g + 513, :], writes=[xb])
            norm_transpose(xb.t[:], xb, 0, std_evac(lambda k: aT.t[:, k, 0:128]), lambda k: aTk[0][k])
            for c in range(4):
                pg, pu = PS[2], PS[3]
                for (pb_, col0) in ((pg, 1280 + c * 128), (pu, 1792 + c * 128)):
                    for k in range(8):
                        K.op("pe", lambda k=k, pb_=pb_, col0=col0: nc.tensor.matmul(
                            pb_.t[:, 0:128], lhsT=wmi.t[:, k, col0:col0 + 128], rhs=aT.t[:, k, 0:128],
                            start=(k == 0), stop=(k == 7)), reads=[wmi] + aTr(k, 0), writes=[pb_], inc=(k == 7))
                K.op("dve", lambda: nc.vector.tensor_copy(out=gcs.t[:, 0:32], in_=pg.t[:, 0:32]), reads=[pg], writes=[gcs])
                K.op("dve", lambda c=c: nc.vector.tensor_tensor(out=vb.t[:, c, :], in0=gcs.t[:, 0:32], in1=pu.t[:, 0:32],
                                                                op=ALU.mult), reads=[gcs, pu], writes=[vb])

            S_B = [PS[0], PS[1], PS[2]]
            s_i = [0]
            pt_i = [0]
            mo_i = [0]
            for g in range(NG):
                t0 = g * GT
                load_rope(t0)
                items = []
                for ti in range(4):
                    xb = next_xin()
                    K.dma("sp", xb.t[:], x_d[t0 + ti * 128:t0 + (ti + 1) * 128, :], writes=[xb])
                    items.append((xb.t[:], xb, 0, std_evac(lambda k, ti=ti: aT.t[:, k, ti * 128:(ti + 1) * 128]),
                                  lambda k, ti=ti: aTk[ti][k]))
                norm_transpose_multi(items)
                for tp in range(2):
                    tis = (2 * tp, 2 * tp + 1)
                    for i, ti in enumerate(tis):
                        pq = PS[4 + i]
                        for k in range(8):
                            K.op("pe", lambda k=k, ti=ti, pq=pq: nc.tensor.matmul(
                                pq.t[:], lhsT=aT.t[:, k, ti * 128:(ti + 1) * 128], rhs=wmi.t[:, k, 0:512],
                                start=(k == 0), stop=(k == 7)), reads=aTr(k, ti) + [wmi], writes=[pq], inc=(k == 7))
                    hros = head_norm_rope_multi([(PS[4 + i], 8, qg_bc, ti) for i, ti in enumerate(tis)])
                    for i, ti in enumerate(tis):
                        ptq = PS[6 + i]
                        for j in range(4):
                            K.op("pe", lambda j=j, ptq=ptq, i=i: nc.tensor.transpose(
                                ptq.t[:, j * 128:(j + 1) * 128], hros[i].t[:, j * 128:(j + 1) * 128], ident.t[:]),
                                reads=[hros[i], ident], writes=[ptq], inc=(j == 3))
                        for hi2 in range(2):
                            K.op("dve", lambda ti=ti, hi2=hi2, ptq=ptq: nc.vector.tensor_copy(
                                out=qT.t[hi2 * 64:(hi2 + 1) * 64, 4 * hi2:4 * hi2 + 4, ti * 128:(ti + 1) * 128],
                                in_=ptq.t[hi2 * 64:(hi2 + 1) * 64, :].rearrange("p (j n) -> p j n", n=128)),
                                reads=[ptq], writes=[qT])
                for c in range(4):
                    pg, pu, pgb = PS[4], PS[5], PS[6]
                    for (pb_, col0) in ((pg, 1280 + c * 128), (pu, 1792 + c * 128), (pgb, 768 + c * 128)):
                        for k in range(8):
                            K.op("pe", lambda k=k, pb_=pb_, col0=col0: nc.tensor.matmul(
                                pb_.t[:], lhsT=wmi.t[:, k, col0:col0 + 128], rhs=aT.t[:, k, :],
                                start=(k == 0), stop=(k == 7)), reads=[wmi] + aTr(k), writes=[pb_], inc=(k == 7))
                    K.op("dve", lambda: nc.vector.tensor_copy(out=gcs.t[:], in_=pg.t[:]), reads=[pg], writes=[gcs])
                    K.op("dve", lambda: nc.vector.tensor_tensor(out=vext.t[:, 1:GT + 1], in0=gcs.t[:], in1=pu.t[:], op=ALU.mult),
                         reads=[gcs, pu], writes=[vext])
                    if g == 0:
                        K.op("pool", lambda: nc.gpsimd.memset(vext.t[:, 0:1], 0.0), writes=[vext])
                    else:
                        K.op("pool", lambda c=c, g=g: nc.gpsimd.tensor_copy(
                            out=vext.t[:, 0:1], in_=vb.t[:, c, 2 * (g - 1):2 * (g - 1) + 1]), reads=[vb], writes=[vext])
                    if g == NG - 1:
                        K.op("pool", lambda: nc.gpsimd.memset(vext.t[:, GT + 1:GT + 2], 0.0), writes=[vext])
                    else:
                        K.op("pool", lambda c=c, g=g: nc.gpsimd.tensor_copy(
                            out=vext.t[:, GT + 1:GT + 2], in_=vb.t[:, c, 2 * g + 1:2 * g + 2]), reads=[vb], writes=[vext])
                    K.op("dve", lambda: nc.vector.tensor_copy(out=gbs.t[:], in_=pgb.t[:]), reads=[pgb], writes=[gbs])
                    K.op("dve", lambda c=c: nc.vector.tensor_scalar(
                        out=cacc.t[:], in0=vext.t[:, 1:GT + 1], scalar1=cwc.t[:, 1, c:c + 1], scalar2=None, op0=ALU.mult),
                        reads=[vext, cwc], writes=[cacc])
                    K.op("dve", lambda c=c: nc.vector.scalar_tensor_tensor(
                        out=cacc.t[:], in0=vext.t[:, 0:GT], scalar=cwc.t[:, 0, c:c + 1], in1=cacc.t[:],
                        op0=ALU.mult, op1=ALU.add), reads=[vext, cwc, cacc], writes=[cacc])
                    K.op("dve", lambda c=c: nc.vector.scalar_tensor_tensor(
                        out=cacc.t[:], in0=vext.t[:, 2:GT + 2], scalar=cwc.t[:, 2, c:c + 1], in1=cacc.t[:],
                        op0=ALU.mult, op1=ALU.add), reads=[vext, cwc, cacc], writes=[cacc])
                    K.op("pool", lambda c=c: nc.gpsimd.tensor_tensor(out=convT.t[:, c, :], in0=cacc.t[:], in1=gbs.t[:],
                                                                     op=ALU.mult), reads=[cacc, gbs], writes=[convT])
                for j in range(4):
                    for hi in range(2):
                        r0 = hi * 64
                        oacc = PS[6 + (j * 2 + hi) % 2]
                        LA = 2
                        sb_of = {}
                        pt_of = {}

                        def issue_s(kb):
                            sbk = S_B[s_i[0] % 3]
                            s_i[0] += 1
                            sb_of[kb] = sbk
                            K.op("pe", lambda: nc.tensor.matmul(
                                sbk.t[:], lhsT=KT.t[:, kb * 128:(kb + 1) * 128], rhs=qT.t[:, j + 4 * hi, :],
                                start=True, stop=True), reads=[KT, qT], writes=[sbk])
                            ptb = PT[pt_i[0] % NPT]
                            pt_i[0] += 1
                            pt_of[kb] = ptb
                            K.op("act", lambda: nc.scalar.activation(out=ptb.t[:], in_=sbk.t[:], func=AF.Exp),
                                 reads=[sbk], writes=[ptb])

                        for kb in range(min(LA, NKB)):
                            issue_s(kb)
                        for kb in range(NKB):
                            if kb + LA < NKB:
                                issue_s(kb + LA)
                            ptb = pt_of.pop(kb)
                            K.op("pe", lambda kb=kb, ptb=ptb: nc.tensor.matmul(
                                oacc.t[:], lhsT=VX.t[:, kb, r0:r0 + 128], rhs=ptb.t[:],
                                start=(kb == 0), stop=(kb == NKB - 1)), reads=[VX, ptb], writes=[oacc])
                        d0 = 64 - r0
                        ds_ = dsh[hi]
                        K.op("dve", lambda: nc.vector.tensor_copy(out=ds_.t[r0:r0 + 64, :], in_=oacc.t[d0:d0 + 64, :]),
                             reads=[oacc], writes=[ds_])
                        K.op("dve", lambda: nc.vector.reciprocal(out=ds_.t[r0:r0 + 64, :], in_=ds_.t[r0:r0 + 64, :]),
                             reads=[ds_], writes=[ds_])
                        K.op("dve", lambda: nc.vector.tensor_tensor(
                            out=attnT.t[r0:r0 + 64, j, :], in0=oacc.t[r0:r0 + 64, :], in1=ds_.t[r0:r0 + 64, :], op=ALU.mult),
                            reads=[oacc, ds_], writes=[attnT])
                for ti in range(4):
                    xob = xo[xo_i[0] % 2]
                    xo_i[0] += 1
                    K.dma("sp", xob.t[:], x_d[t0 + ti * 128:t0 + (ti + 1) * 128, :], writes=[xob])
                    for half in range(2):
                        pm = PS[3 + mo_i[0] % 3]
                        mo_i[0] += 1
                        for k in range(8):
                            src, w_ = (attnT, woA) if k < 4 else (convT, woC)
                            K.op("pe", lambda k=k, src=src, w_=w_, pm=pm, ti=ti, half=half: nc.tensor.matmul(
                                pm.t[:], lhsT=src.t[:, k % 4, ti * 128:(ti + 1) * 128],
                                rhs=w_.t[:, k % 4, half * 512:(half + 1) * 512], start=(k == 0), stop=(k == 7)),
                                reads=[src, w_], writes=[pm], inc=(k == 7))
                        K.op("dve", lambda pm=pm, xob=xob, half=half: nc.vector.tensor_tensor(
                            out=xob.t[:, half * 512:(half + 1) * 512], in0=pm.t[:], in1=xob.t[:, half * 512:(half + 1) * 512],
                            op=ALU.add), reads=[pm, xob], writes=[xob])
                    K.dma("pool", xmid_d[t0 + ti * 128:t0 + (ti + 1) * 128, :], xob.t[:], reads=[xob], writes=[xmid_b])
            K.barrier()
        if stop_after == "xmid":
            finish()
            return nc

        with contextlib.ExitStack() as ph:
            sbp = mk_sb(ph)
            NWS = 3
            wslot = [sbp("wslot%d" % i, [128, 2048], BF16) for i in range(NWS)]
            ws_i = [0]
            ds_i = [0]
            FF = {}
            sg = [sbp("sg%d" % i, [128, GT]) for i in range(2)]
            sg_i = [0]
            xo = [sbp("xo%d" % i, [128, D]) for i in range(2)]
            xo_i = [0]
            gu_i = [0]
            dn_i = [0]

            def gate_up(src_ap, src_buf, j, at=None, atr=None):
                wsl = wslot[ws_i[0] % NWS]
                ws_i[0] += 1
                K.dma("sp", wsl.t[:], src_ap, reads=[src_buf], writes=[wsl])
                pg, pu = (PS[4], PS[5]) if gu_i[0] % 2 == 0 else (PS[6], PS[7])
                gu_i[0] += 1
                for m, pb_ in enumerate((pg, pu)):
                    for k in range(8):
                        K.op("pe", lambda k=k, m=m, pb_=pb_: nc.tensor.matmul(
                            pb_.t[:], lhsT=wsl.t[:, (m * 8 + k) * 128:(m * 8 + k + 1) * 128], rhs=(at or aT).t[:, k, :],
                            start=(k == 0), stop=(k == 7)), reads=[wsl] + (atr or aTr)(k), writes=[pb_], inc=(k == 7))
                sgb = sg[sg_i[0] % 2]
                sg_i[0] += 1
                K.op("act", lambda: nc.scalar.activation(out=sgb.t[:], in_=pg.t[:], func=AF.Silu), reads=[pg], writes=[sgb])
                K.op("dve", lambda: nc.vector.tensor_tensor(out=FF["hT"].t[:, j, :], in0=sgb.t[:], in1=pu.t[:], op=ALU.mult),
                     reads=[sgb, pu], writes=[FF["hTk"][j]])

            def down(src_fn, src_buf, jn, consume):
                for half in range(2):
                    dsl = FF["dslot"][ds_i[0] % 2]
                    ds_i[0] += 1
                    K.dma("sp", dsl.t[:, 0:jn, :], src_fn(half), reads=[src_buf], writes=[dsl])
                    for ti in range(4):
                        pd = PS[dn_i[0] % FF["ndn"]]
                        dn_i[0] += 1
                        for jj in range(jn):
                            K.op("pe", lambda jj=jj, pd=pd, ti=ti, dsl=dsl: nc.tensor.matmul(
                                pd.t[:], lhsT=FF["hT"].t[:, jj, ti * 128:(ti + 1) * 128], rhs=dsl.t[:, jj, :],
                                start=(jj == 0), stop=(jj == jn - 1)), reads=[FF["hTk"][jj], dsl], writes=[pd], inc=(jj == jn - 1))
                        consume(ti, half, pd)

            xacc = sbp("xacc", [128, 4, D])
            xk = [[Buf("xacc_%d_%d" % (ti, h), xacc.t) for h in range(2)] for ti in range(4)]
            xall = [b for r in xk for b in r]
            phB = contextlib.ExitStack()
            sbB = mk_sb(phB)
            FF["dslot"] = [sbB("dslotB%d" % i, [128, 22, 512], BF16) for i in range(2)]
            FF["hT"] = sbB("hTB", [128, 22, GT], BF16)
            FF["hTk"] = [Buf("hTB_%d" % j, FF["hT"].t) for j in range(22)]
            FF["ndn"] = 4
            for g in range(NG):
                t0 = g * GT
                K.dma("sp", xacc.t[:], xmid_d[t0:t0 + GT, :].rearrange("(t p) n -> p t n", p=128),
                      reads=[xmid_b], writes=xall)
                norm_transpose_multi([(xacc.t[:, ti, :], xk[ti], 1, std_evac(lambda k, ti=ti: aT.t[:, k, ti * 128:(ti + 1) * 128]),
                                       lambda k, ti=ti: aTk[ti][k]) for ti in range(4)])
                for j in range(22):
                    gate_up(wgu_s[j], wgu_b, j)

                def cons_b(ti, half, pd):
                    K.op("dve", lambda: nc.vector.tensor_tensor(
                        out=xacc.t[:, ti, half * 512:(half + 1) * 512], in0=pd.t[:],
                        in1=xacc.t[:, ti, half * 512:(half + 1) * 512], op=ALU.add), reads=[pd, xk[ti][half]], writes=[xk[ti][half]])

                down(lambda half: wd_s[:, :, half * 512:(half + 1) * 512], wd_b, 22, cons_b)
                K.dma("pool", x1_d[t0:t0 + GT, :].rearrange("(t p) n -> p t n", p=128), xacc.t[:], reads=xall, writes=[x1_b])
            if stop_after == "x1":
                finish()
                return nc
            K.barrier()
            phB.close()
            FF["dslot"] = [sbp("dslotC%d" % i, [128, 11, 512], BF16) for i in range(2)]
            FF["hT"] = sbp("hTC", [128, 11, GT], BF16)
            FF["hTk"] = [Buf("hTC_%d" % j, FF["hT"].t) for j in range(11)]
            FF["ndn"] = 2

            pwb = sbp("pwb", [128, 8, 256], BF16)
            rwb = sbp("rwb", [128, 8, NE], BF16)
            K.dma("sp", pwb.t[:], pwb_s[:, :, :], reads=[wsm_b], writes=[pwb])
            K.dma("sp", rwb.t[:], rwb_s[:, :, :], reads=[wsm_b], writes=[rwb])
            fg_bc = bc_row("fg_bc", fg_d, D, sbp)
            EW = GT + 16
            a1T = sbp("a1T", [128, 8, EW])
            a1k = [Buf("a1T_%d" % k, a1T.t) for k in range(8)]
            T1 = sbp("T1", [128, 2, EW])
            T2 = sbp("T2", [128, 2, EW])
            T3 = sbp("T3", [128, 2, EW])
            pT = sbp("pT", [128, 8, GT], BF16)
            lg = sbp("lg", [128, 32])
            mx = sbp("mx", [128, 32])
            nm1 = sbp("nm1", [128, 1])
            msk = sbp("msk", [128, 32])
            exl = sbp("exl", [128, 32])
            gsum = sbp("gsum", [128, 4])
            gates = sbp("gates", [128, 4, NE])
            xacc2 = sbp("xacc2", [128, 4, D])
            xk2 = [[Buf("xacc2_%d_%d" % (ti, h), xacc2.t) for h in range(2)] for ti in range(4)]
            aT2 = sbp("aT2", [128, 8, GT], BF16)
            aT2k = [[Buf("aT2_%d_%d" % (ti, k), aT2.t) for k in range(8)] for ti in range(4)]
            gates2 = sbp("gates2", [128, 4, NE])
            print("SBUF remaining after phase C alloc:", nc.sbuf_bytes_remaining)
            XA = [xacc, xacc2]
            XK = [xk, xk2]
            ATT = [aT, aT2]
            ATK = [aTk, aT2k]
            GA = [gates, gates2]
            TRB = (PS[2], PS[3])

            def make_prologue(g):
                par = g % 2
                xa, xkk, at, atk, ga = XA[par], XK[par], ATT[par], ATK[par], GA[par]
                xall_ = [b for r in xkk for b in r]
                t0 = g * GT
                st = {}

                def f32_evac(dst_fn):
                    def ev(k, p_ap, sc_ap, b_ap, eng, reads, writes):
                        if eng == "dve":
                            K.op("dve", lambda: nc.vector.tensor_scalar(out=dst_fn(k), in0=p_ap, scalar1=sc_ap, scalar2=b_ap,
                                                                        op0=ALU.mult, op1=ALU.add), reads=reads, writes=writes)
                        else:
                            K.op("act", lambda: nc.scalar.activation(out=dst_fn(k), in_=p_ap, func=AF.Identity,
                                                                     scale=sc_ap, bias=b_ap), reads=reads, writes=writes)
                    return ev

                def halo_evac(k, p_ap, sc_ap, b_ap, eng, reads, writes):
                    for (c0_, c1_, d0, valid) in ((0, 8, 0, g > 0), (8, 16, GT + 8, g < NG - 1)):
                        if valid:
                            K.op("dve", lambda c0_=c0_, c1_=c1_, d0=d0: nc.vector.tensor_scalar(
                                out=a1T.t[:, k, d0:d0 + 8], in0=p_ap[:, c0_:c1_], scalar1=sc_ap, scalar2=b_ap,
                                op0=ALU.mult, op1=ALU.add), reads=reads, writes=writes)
                        else:
                            K.op("pool", lambda d0=d0: nc.gpsimd.memset(a1T.t[:, k, d0:d0 + 8], 0.0), writes=writes)

                def s_load():
                    K.dma("sp", xa.t[:], x1_d[t0:t0 + GT, :].rearrange("(t p) n -> p t n", p=128), reads=[x1_b], writes=xall_)
                    xb = next_xin()
                    st["xb"] = xb
                    K.op("pool", lambda: nc.gpsimd.memset(xb.t[:], 0.0), writes=[xb])
                    if g > 0:
                        K.dma("sp", xb.t[0:8, :], x1_d[t0 - 8:t0, :], reads=[x1_b], writes=[xb])
                    if g < NG - 1:
                        K.dma("sp", xb.t[8:15, :], x1_d[t0 + GT:t0 + GT + 7, :], reads=[x1_b], writes=[xb])
                    st["items1"] = [(xb.t[:], [xb], 2, halo_evac, lambda k: a1k[k])] + [
                        (xa.t[:, ti, :], xkk[ti], 2, f32_evac(lambda k, ti=ti: a1T.t[:, k, 8 + ti * 128:8 + (ti + 1) * 128]),
                         lambda k: a1k[k]) for ti in range(4)]
                    st["items2"] = [(xa.t[:, ti, :], xkk[ti], 3, std_evac(lambda k, ti=ti: at.t[:, k, ti * 128:(ti + 1) * 128]),
                                     lambda k, ti=ti: atk[ti][k]) for ti in range(4)]

                def mk_stats(key, skey):
                    def a():
                        ss = []
                        for _ in st[key]:
                            s_ = ssq[ssq_i[0] % 8]
                            ssq_i[0] += 1
                            ss.append(s_)
                            K.op("pool", lambda s_=s_: nc.gpsimd.memset(s_.t[:], 0.0), writes=[s_])
                        st[skey] = ss
                        for (x_ap, x_buf, mi, evac, dbf), s_ in zip(st[key], ss):
                            K.op("act", lambda x_ap=x_ap, s_=s_: nc.scalar.activation(out=junk.t[:], in_=x_ap, func=AF.Square,
                                                                                    accum_out=s_.t[:, 0:1]),
                                 reads=list(x_buf) + [s_], writes=[junk, s_])

                    def b():
                        for s_ in st[skey]:
                            K.op("act", lambda s_=s_: nc.scalar.activation(out=s_.t[:, 1:2], in_=s_.t[:, 0:1], func=AF.Sqrt,
                                                                           bias=epsb.t[:], scale=1.0 / D),
                                 reads=[s_, epsb], writes=[s_])

                    def c():
                        for s_ in st[skey]:
                            K.op("dve", lambda s_=s_: nc.vector.reciprocal(out=s_.t[:, 1:2], in_=s_.t[:, 1:2]),
                                 reads=[s_], writes=[s_])
                    return a, b, c

                def mk_scale(key, skey, idxs, xkey):
                    def f():
                        xbs = st.setdefault(xkey, {})
                        for i in idxs:
                            x_ap, x_buf, mi, evac, dbf = st[key][i]
                            s_ = st[skey][i]
                            xb_ = xn[xn_i[0] % 4]
                            xn_i[0] += 1
                            xbs[i] = xb_
                            K.op("act", lambda xb_=xb_, x_ap=x_ap, s_=s_: nc.scalar.activation(
                                out=xb_.t[:], in_=x_ap, func=AF.Identity, scale=s_.t[:, 1:2]),
                                reads=list(x_buf) + [s_], writes=[xb_])
                    return f

                def mk_tr(key, idxs, xkey):
                    def f():
                        for i in idxs:
                            x_ap, x_buf, mi, evac, dbf = st[key][i]
                            xb_ = st[xkey][i]
                            for half in range(2):
                                pb = TRB[half]
                                for kk in range(4):
                                    k = half * 4 + kk
                                    K.op("pe", lambda k=k, kk=kk, pb=pb, xb_=xb_: nc.tensor.transpose(
                                        pb.t[:, kk * 128:(kk + 1) * 128], xb_.t[:, k * 128:(k + 1) * 128], ident.t[:]),
                                        reads=[xb_, ident], writes=[pb], inc=(kk == 3))
                                for kk in range(4):
                                    k = half * 4 + kk
                                    evac(k, pb.t[:, kk * 128:(kk + 1) * 128], gsc.t[:, mi, k:k + 1], shc.t[:, mi, k:k + 1],
                                         "dve" if half == 0 else "act", [pb, gsc, shc], [dbf(k)])
                    return f

                def s_pool():
                    if g < NG - 1:
                        K.op("pool", lambda: nc.gpsimd.memset(a1T.t[:, :, GT + 15:GT + 16], 0.0), writes=a1k)
                    for gi, w in enumerate((2, 4, 8, 16)):
                        E = a1T.t[:, 2 * gi:2 * gi + 2, :]
                        eng = nc.vector
                        rd = [a1k[2 * gi], a1k[2 * gi + 1]]
                        if gi == 0:
                            K.op("dve", lambda E=E: eng.tensor_tensor(out=T1.t[:, :, 8:GT + 8], in0=E[:, :, 7:GT + 7],
                                                                      in1=E[:, :, 8:GT + 8], op=ALU.add), reads=rd, writes=[T1])
                            W_, Wb = T1.t[:, :, 8:GT + 8], T1
                        else:
                            K.op("dve", lambda E=E: eng.tensor_tensor(out=T1.t[:, :, 0:EW - 1], in0=E[:, :, 0:EW - 1],
                                                                      in1=E[:, :, 1:EW], op=ALU.add), reads=rd, writes=[T1])
                            if gi == 1:
                                K.op("dve", lambda: eng.tensor_tensor(out=T2.t[:, :, 8:GT + 8], in0=T1.t[:, :, 6:GT + 6],
                                                                      in1=T1.t[:, :, 8:GT + 8], op=ALU.add), reads=[T1], writes=[T2])
                                W_, Wb = T2.t[:, :, 8:GT + 8], T2
                            else:
                                K.op("dve", lambda: eng.tensor_tensor(out=T2.t[:, :, 0:EW - 3], in0=T1.t[:, :, 0:EW - 3],
                                                                      in1=T1.t[:, :, 2:EW - 1], op=ALU.add), reads=[T1], writes=[T2])
                                if gi == 2:
                                    K.op("dve", lambda: eng.tensor_tensor(out=T3.t[:, :, 8:GT + 8], in0=T2.t[:, :, 4:GT + 4],
                                                                          in1=T2.t[:, :, 8:GT + 8], op=ALU.add), reads=[T2], writes=[T3])
                                    W_, Wb = T3.t[:, :, 8:GT + 8], T3
                                else:
                                    K.op("dve", lambda: eng.tensor_tensor(out=T3.t[:, :, 0:EW - 7], in0=T2.t[:, :, 0:EW - 7],
                                                                          in1=T2.t[:, :, 4:EW - 3], op=ALU.add), reads=[T2], writes=[T3])
                                    K.op("dve", lambda: eng.tensor_tensor(out=T1.t[:, :, 8:GT + 8], in0=T3.t[:, :, 0:GT],
                                                                          in1=T3.t[:, :, 8:GT + 8], op=ALU.add), reads=[T3], writes=[T1])
                                    W_, Wb = T1.t[:, :, 8:GT + 8], T1
                        K.op("dve", lambda E=E, W_=W_, w=w, gi=gi: eng.scalar_tensor_tensor(
                            out=pT.t[:, 2 * gi:2 * gi + 2, :], in0=W_, scalar=1.0 / w, in1=E[:, :, 8:GT + 8],
                            op0=ALU.mult, op1=ALU.subtract), reads=[Wb] + rd, writes=[pT])
                        for (edge, ok, c0_) in ((0, g == 0, 0), (1, g == NG - 1, GT - 8)):
                            if ok:
                                icv = icnt_bc.t[:, edge * 32 + gi * 8:edge * 32 + gi * 8 + 8].unsqueeze(1).to_broadcast([128, 2, 8])
                                K.op("dve", lambda W_=W_, c0_=c0_, icv=icv: nc.vector.tensor_tensor(
                                    out=W_[:, :, c0_:c0_ + 8], in0=W_[:, :, c0_:c0_ + 8], in1=icv, op=ALU.mult),
                                    reads=[Wb, icnt_bc], writes=[Wb])
                                K.op("dve", lambda W_=W_, c0_=c0_, E=E, gi=gi: nc.vector.tensor_tensor(
                                    out=pT.t[:, 2 * gi:2 * gi + 2, c0_:c0_ + 8], in0=W_[:, :, c0_:c0_ + 8],
                                    in1=E[:, :, 8 + c0_:16 + c0_], op=ALU.subtract), reads=[Wb] + rd, writes=[pT])

                def s_poolmm():
                    for ti in range(4):
                        for bnk in range(2):
                            pp = TRB[bnk]
                            for gg in range(2):
                                gi = bnk * 2 + gg
                                for kk in range(2):
                                    K.op("pe", lambda gi=gi, kk=kk, gg=gg, pp=pp, ti=ti: nc.tensor.matmul(
                                        pp.t[:, gg * 256:(gg + 1) * 256], lhsT=pT.t[:, 2 * gi + kk, ti * 128:(ti + 1) * 128],
                                        rhs=pwb.t[:, 2 * gi + kk, :], start=(kk == 0), stop=(kk == 1)),
                                        reads=[pT, pwb], writes=[pp], inc=(gg == 1 and kk == 1))
                            K.op("dve", lambda pp=pp, ti=ti, bnk=bnk: nc.vector.tensor_tensor(
                                out=xa.t[:, ti, bnk * 512:(bnk + 1) * 512], in0=pp.t[:],
                                in1=xa.t[:, ti, bnk * 512:(bnk + 1) * 512], op=ALU.add),
                                reads=[pp, xkk[ti][bnk]], writes=[xkk[ti][bnk]])

                def s_router():
                    pr = TRB[0]
                    for ti in range(4):
                        for k in range(8):
                            K.op("pe", lambda k=k, ti=ti: nc.tensor.matmul(
                                pr.t[:, ti * NE:(ti + 1) * NE], lhsT=at.t[:, k, ti * 128:(ti + 1) * 128], rhs=rwb.t[:, k, :],
                                start=(k == 0), stop=(k == 7)), reads=[atk[ti][k], rwb], writes=[pr], inc=(k == 7 and ti == 3))

                def s_gate1():
                    pr = TRB[0]
                    v3 = lambda b: b.t[:, 0:4 * NE].rearrange("p (t e) -> p t e", e=NE)
                    K.op("dve", lambda: nc.vector.tensor_tensor(
                        out=v3(lg), in0=pr.t[:, 0:4 * NE].rearrange("p (t e) -> p t e", e=NE),
                        in1=rb_bc.t[:].unsqueeze(1).to_broadcast([128, 4, NE]), op=ALU.add), reads=[pr, rb_bc], writes=[lg])
                    for ti in range(4):
                        K.op("dve", lambda ti=ti: nc.vector.max(out=mx.t[:, ti * NE:(ti + 1) * NE], in_=lg.t[:, ti * NE:(ti + 1) * NE]),
                             reads=[lg], writes=[mx])
                    K.op("dve", lambda: nc.vector.tensor_tensor(
                        out=v3(msk), in0=v3(lg), in1=v3(mx)[:, :, 1:2].to_broadcast([128, 4, NE]), op=ALU.is_ge),
                        reads=[lg, mx], writes=[msk])
                    K.op("dve", lambda: nc.vector.tensor_tensor(
                        out=v3(exl), in0=v3(lg), in1=v3(mx)[:, :, 0:1].to_broadcast([128, 4, NE]), op=ALU.subtract),
                        reads=[lg, mx], writes=[exl])

                def s_gate2():
                    K.op("act", lambda: nc.scalar.activation(out=exl.t[:, 0:4 * NE], in_=exl.t[:, 0:4 * NE], func=AF.Exp),
                         reads=[exl], writes=[exl])

                def s_gate3():
                    v3 = lambda b: b.t[:, 0:4 * NE].rearrange("p (t e) -> p t e", e=NE)
                    K.op("dve", lambda: nc.vector.tensor_tensor(out=exl.t[:, 0:4 * NE], in0=exl.t[:, 0:4 * NE],
                                                                in1=msk.t[:, 0:4 * NE], op=ALU.mult),
                         reads=[exl, msk], writes=[exl])
                    K.op("dve", lambda: nc.vector.tensor_reduce(out=gsum.t[:, 0:4], in_=v3(exl), axis=AX.X, op=ALU.add),
                         reads=[exl], writes=[gsum])
                    K.op("dve", lambda: nc.vector.reciprocal(out=gsum.t[:, 0:4], in_=gsum.t[:, 0:4]), reads=[gsum], writes=[gsum])
                    K.op("dve", lambda: nc.vector.tensor_tensor(
                        out=ga.t[:], in0=v3(exl), in1=gsum.t[:, 0:4].unsqueeze(2).to_broadcast([128, 4, NE]), op=ALU.mult),
                        reads=[exl, gsum], writes=[ga])

                a1, b1, c1_ = mk_stats("items1", "ss1")
                a2, b2, c2_ = mk_stats("items2", "ss2")
                return [s_load, a1, b1, c1_,
                        mk_scale("items1", "ss1", [0, 1, 2, 3], "xb1"), mk_tr("items1", [0, 1, 2, 3], "xb1"),
                        mk_scale("items1", "ss1", [4], "xb1"), mk_tr("items1", [4], "xb1"),
                        s_pool, s_poolmm, a2, b2, c2_,
                        mk_scale("items2", "ss2", [0, 1, 2, 3], "xb2"), mk_tr("items2", [0, 1, 2, 3], "xb2"),
                        s_router, s_gate1, s_gate2, s_gate3]

            for stg in make_prologue(0):
                stg()
            for g in range(NG):
                t0 = g * GT
                par = g % 2
                xa, xkk, at, atk, ga = XA[par], XK[par], ATT[par], ATK[par], GA[par]
                nxt = make_prologue(g + 1) if g + 1 < NG else []
                atr_ = lambda k, atk=atk: [atk[t][k] for t in range(4)]
                cc = 0
                for e in range(NE):
                    for j in range(11):
                        gate_up(egu_s[e * 11 + j], egu_b, j, at=at, atr=atr_)
                        cc += 1
                        if cc % 4 == 0 and nxt:
                            nxt.pop(0)()

                    def cons_c(ti, half, pd, e=e):
                        K.op("dve", lambda: nc.vector.scalar_tensor_tensor(
                            out=xa.t[:, ti, half * 512:(half + 1) * 512], in0=pd.t[:], scalar=ga.t[:, ti, e:e + 1],
                            in1=xa.t[:, ti, half * 512:(half + 1) * 512], op0=ALU.mult, op1=ALU.add),
                            reads=[pd, ga, xkk[ti][half]], writes=[xkk[ti][half]])

                    down(lambda half, e=e: ed_s[e, :, :, half * 512:(half + 1) * 512], ed_b, 11, cons_c)
                while nxt:
                    nxt.pop(0)()
                for ti in range(4):
                    s = rms_rstd(xa.t[:, ti, :], xkk[ti])
                    xob = xo[xo_i[0] % 2]
                    xo_i[0] += 1
                    K.op("dve", lambda s=s, xob=xob, ti=ti: nc.vector.scalar_tensor_tensor(
                        out=xob.t[:], in0=xa.t[:, ti, :], scalar=s.t[:, 1:2], in1=fg_bc.t[:], op0=ALU.mult, op1=ALU.mult),
                        reads=xkk[ti] + [s, fg_bc], writes=[xob])
                    K.dma("pool", out_d[t0 + ti * 128:t0 + (ti + 1) * 128, :], xob.t[:], reads=[xob], writes=[out_b])
            finish()
    return nc


def host_consts():
    ident = np.eye(128, dtype=np.float32)
    t = np.arange(SEQ)
    row = (t // 64).astype(np.float32)
    col = (t % 64).astype(np.float32)
    freqs = (10000.0 ** (-np.arange(0, 32, 2, dtype=np.float32) / 32)).astype(np.float32)
    ang = np.stack([row[:, None] * freqs, col[:, None] * freqs], axis=1).astype(np.float32)
    cos = np.cos(ang).astype(np.float32).reshape(SEQ, 32)
    sin = np.sin(ang).astype(np.float32).reshape(SEQ, 32)
    icnt = np.ones((2, 4, 8), np.float32)
    for gi, w in enumerate((2, 4, 8, 16)):
        for i in range(8):
            tt = i
            icnt[0, gi, i] = 1.0 / (min(tt + w - w // 2, SEQ) - max(tt - w // 2, 0))
            tt = SEQ - 8 + i
            icnt[1, gi, i] = 1.0 / (min(tt + w - w // 2, SEQ) - max(tt - w // 2, 0))
    return ident, cos, sin, icnt


_NC_CACHE = {}


def make_in_maps(inputs):
    ident, cos, sin, icnt = host_consts()
    f = lambda a: np.ascontiguousarray(np.asarray(a, dtype=np.float32))
    shared = {
        "c_ctx": f(inputs["c_ctx"]), "w_mod": f(inputs["w_mod"]), "b_mod": f(inputs["b_mod"]),
        "norm_g": f(inputs["norm_g"]), "final_norm_g": f(inputs["final_norm_g"]),
        "w_mix_in": f(inputs["w_mix_in"][0]), "q_norm_g": f(inputs["q_norm_g"][0]),
        "k_norm_g": f(inputs["k_norm_g"][0]), "conv_w": f(inputs["conv_w"][0]),
        "w_mix_out": f(inputs["w_mix_out"][0]), "ffn_w_gate": f(inputs["ffn_w_gate"][0]),
        "ffn_w_up": f(inputs["ffn_w_up"][0]), "ffn_w_down": f(inputs["ffn_w_down"][0]),
        "pool_w": f(inputs["pool_w"][0]), "pool_scale": f(inputs["pool_scale"][0]),
        "router_w": f(inputs["router_w"][0]), "router_b": f(inputs["router_b"][0]),
        "exp_w_gate": f(inputs["exp_w_gate"][0]), "exp_w_up": f(inputs["exp_w_up"][0]),
        "exp_w_down": f(inputs["exp_w_down"][0]),
        "ident": ident, "rope_cos": cos, "rope_sin": sin, "pool_icnt": icnt.reshape(64),
    }
    x = np.asarray(inputs["x"], dtype=np.float32)
    c = np.asarray(inputs["c"], dtype=np.float32)
    ctx = np.asarray(inputs["ctx"], dtype=np.float32)
    maps = []
    for b in range(NCORES):
        m = dict(shared)
        m["x"] = np.ascontiguousarray(x[b])
        m["c"] = np.ascontiguousarray(c[b])
        m["ctx"] = np.ascontiguousarray(ctx[b])
        maps.append(m)
    return maps


def kernel(**inputs):
    if "nc" not in _NC_CACHE:
        _NC_CACHE["nc"] = build(False)
    nc = _NC_CACHE["nc"]
    maps = make_in_maps(inputs)
    res = run_bass_kernel_spmd(nc, maps, core_ids=list(range(NCORES)))
    return np.stack([np.asarray(r["out"], dtype=np.float32) for r in res.results], axis=0)
```
